# Optimizing a Trainium2 kernel written in Bass

```python
import math
import jax, jax.numpy as jnp
from jax import lax
import numpy as np

D_MODEL = 2048
BATCH = 4
SEQ = 4096
DEPTH = 2

GRID_W = 64
CTX_LEN = 256
CONV_DIM = 1024
CONV_K = 3
ATT_HEADS = 8
ATT_KV_HEADS = 2
HEAD_DIM = 128
MLA_HEADS = 16
Q_LORA = 512
KV_LORA = 512
QK_NOPE = 128
QK_ROPE = 64
V_DIM = 128
N_EXPERTS = 64
TOP_K = 6
N_GROUPS = 8
TOPK_GROUPS = 4
EXPERT_FF = 512
SHARED_FF = 512
ROUTED_SCALE = 2.5
MOE_BLOCK = 256
Q_BLOCK = 128
ROPE_THETA = 10000.0
LN_EPS = 1e-5
RMS_EPS = 1e-6
DN_ALPHA = (2 * DEPTH) ** 0.25
DN_BETA = (8 * DEPTH) ** -0.25

kernel_name = 'hybrid_conv_gqa_mla_moe_diffusion_trunk'


def rms_norm(x, g):
    xf = x.astype(jnp.float32)
    y = xf * lax.rsqrt(jnp.mean(xf * xf, axis=-1, keepdims=True) + RMS_EPS)
    return (y * g.astype(jnp.float32)).astype(x.dtype)


def layer_norm(x, g, b):
    xf = x.astype(jnp.float32)
    mu = jnp.mean(xf, axis=-1, keepdims=True)
    xc = xf - mu
    var = jnp.mean(xc * xc, axis=-1, keepdims=True)
    y = xc * lax.rsqrt(var + LN_EPS) * g.astype(jnp.float32) + b.astype(jnp.float32)
    return y.astype(x.dtype)


def axial_rope(n_tok, rot_dim):
    rows = n_tok // GRID_W
    n_freq = rot_dim // 4
    inv = ROPE_THETA ** (-jnp.arange(n_freq, dtype=jnp.float32) / n_freq)
    row = jnp.repeat(jnp.arange(rows, dtype=jnp.float32), GRID_W)
    col = jnp.tile(jnp.arange(GRID_W, dtype=jnp.float32), rows)
    ang = jnp.concatenate([row[:, None] * inv, col[:, None] * inv], axis=-1)
    return jnp.cos(ang), jnp.sin(ang)


def apply_rope(x, cos, sin):
    xf = x.astype(jnp.float32)
    x1, x2 = jnp.split(xf, 2, axis=-1)
    c = cos[:, None, :]
    s = sin[:, None, :]
    return jnp.concatenate([x1 * c - x2 * s, x2 * c + x1 * s], axis=-1).astype(x.dtype)


def attend(q, k, v, scale):
    s = jnp.einsum('bqhgd,bkhd->bhgqk', q, k, preferred_element_type=jnp.float32) * scale
    p = jax.nn.softmax(s, axis=-1).astype(v.dtype)
    o = jnp.einsum('bhgqk,bkhd->bqhgd', p, v)
    return o.reshape(o.shape[0], o.shape[1], -1, o.shape[-1])


def attend_blocked(q, k, v, scale):
    b, s = q.shape[0], q.shape[1]
    nb = s // Q_BLOCK
    qb = jnp.moveaxis(q.reshape(b, nb, Q_BLOCK, *q.shape[2:]), 1, 0)
    ob = lax.map(lambda qq: attend(qq, k, v, scale), qb)
    return jnp.moveaxis(ob, 0, 1).reshape(b, s, ob.shape[3], ob.shape[4])


def short_conv(x, w):
    return lax.conv_general_dilated(
        x, w[:, None, :].astype(x.dtype), window_strides=(1,),
        padding=((CONV_K // 2, CONV_K // 2),),
        dimension_numbers=('NWC', 'WIO', 'NWC'),
        feature_group_count=x.shape[-1])


def conv_attn_mixer(u_lat, u_ctx, w_in, conv_w, q_gain, k_gain, w_out, ctx_out):
    d_q = ATT_HEADS * HEAD_DIM
    d_kv = ATT_KV_HEADS * HEAD_DIM
    kv_lo = 3 * CONV_DIM + d_q
    cuts = [CONV_DIM, 2 * CONV_DIM, 3 * CONV_DIM, kv_lo, kv_lo + d_kv]
    grp = ATT_HEADS // ATT_KV_HEADS
    scale = 1.0 / math.sqrt(HEAD_DIM)

    def heads(t, n, gain):
        return rms_norm(t.reshape(t.shape[0], t.shape[1], n, HEAD_DIM), gain)

    gb, gc, hv, q, k, v = jnp.split(u_lat @ w_in, cuts, axis=-1)
    cos, sin = axial_rope(u_lat.shape[1], HEAD_DIM)
    q = apply_rope(heads(q, ATT_HEADS, q_gain), cos, sin)
    k = apply_rope(heads(k, ATT_KV_HEADS, k_gain), cos, sin)
    v = v.reshape(v.shape[0], v.shape[1], ATT_KV_HEADS, HEAD_DIM)

    if ctx_out:
        gb_c, gc_c, hv_c, q_c, k_c, v_c = jnp.split(u_ctx @ w_in, cuts, axis=-1)
    else:
        k_c, v_c = jnp.split(u_ctx @ w_in[:, kv_lo:], [d_kv], axis=-1)
    k_c = heads(k_c, ATT_KV_HEADS, k_gain)
    v_c = v_c.reshape(v_c.shape[0], v_c.shape[1], ATT_KV_HEADS, HEAD_DIM)

    k_all = jnp.concatenate([k_c, k], axis=1)
    v_all = jnp.concatenate([v_c, v], axis=1)
    q5 = q.reshape(q.shape[0], q.shape[1], ATT_KV_HEADS, grp, HEAD_DIM)
    att = attend_blocked(q5, k_all, v_all, scale)
    att = att.reshape(att.shape[0], att.shape[1], d_q)
    conv = gb * short_conv(gc * hv, conv_w)
    y_lat = jnp.concatenate([conv, att], axis=-1) @ w_out

    y_ctx = None
    if ctx_out:
        q_c = heads(q_c, ATT_HEADS, q_gain).reshape(q_c.shape[0], q_c.shape[1], ATT_KV_HEADS, grp, HEAD_DIM)
        att_c = attend(q_c, k_c, v_c, scale).reshape(q_c.shape[0], q_c.shape[1], d_q)
        conv_c = gb_c * short_conv(gc_c * hv_c, conv_w)
        y_ctx = jnp.concatenate([conv_c, att_c], axis=-1) @ w_out
    return y_lat, y_ctx


def mla_mixer(u_lat, u_ctx, w_down, q_gain, kv_gain, w_uq, w_ukv, w_out, ctx_out):
    scale = 1.0 / math.sqrt(QK_NOPE + QK_ROPE)

    def queries(cq, rope):
        qq = rms_norm(cq, q_gain) @ w_uq
        qq = qq.reshape(cq.shape[0], cq.shape[1], MLA_HEADS, QK_NOPE + QK_ROPE)
        q_nope, q_rope = jnp.split(qq, [QK_NOPE], axis=-1)
        if rope is not None:
            q_rope = apply_rope(q_rope, *rope)
        return jnp.concatenate([q_nope, q_rope], axis=-1)[:, :, :, None, :]

    def keys_values(ckv, kr, rope):
        kv = rms_norm(ckv, kv_gain) @ w_ukv
        kv = kv.reshape(ckv.shape[0], ckv.shape[1], MLA_HEADS, QK_NOPE + V_DIM)
        k_nope, vv = jnp.split(kv, [QK_NOPE], axis=-1)
        kr = kr[:, :, None, :]
        if rope is not None:
            kr = apply_rope(kr, *rope)
        kk = jnp.concatenate([k_nope, jnp.broadcast_to(kr, k_nope.shape[:-1] + (QK_ROPE,))], axis=-1)
        return kk, vv

    rope = axial_rope(u_lat.shape[1], QK_ROPE)
    cq, ckv, kr = jnp.split(u_lat @ w_down, [Q_LORA, Q_LORA + KV_LORA], axis=-1)
    q = queries(cq, rope)
    k, v = keys_values(ckv, kr, rope)

    if ctx_out:
        cq_c, ckv_c, kr_c = jnp.split(u_ctx @ w_down, [Q_LORA, Q_LORA + KV_LORA], axis=-1)
    else:
        ckv_c, kr_c = jnp.split(u_ctx @ w_down[:, Q_LORA:], [KV_LORA], axis=-1)
    k_c, v_c = keys_values(ckv_c, kr_c, None)

    k_all = jnp.concatenate([k_c, k], axis=1)
    v_all = jnp.concatenate([v_c, v], axis=1)
    att = attend_blocked(q, k_all, v_all, scale)
    y_lat = att.reshape(att.shape[0], att.shape[1], MLA_HEADS * V_DIM) @ w_out

    y_ctx = None
    if ctx_out:
        att_c = attend(queries(cq_c, None), k_c, v_c, scale)
        y_ctx = att_c.reshape(att_c.shape[0], att_c.shape[1], MLA_HEADS * V_DIM) @ w_out
    return y_lat, y_ctx


def moe_ffn(t, router_w, router_b, w_gate, w_up, w_down, s_gate, s_up, s_down):
    n_tok, d = t.shape
    logits = jnp.einsum('td,de->te', t, router_w, preferred_element_type=jnp.float32)
    scores = jax.nn.sigmoid(logits)
    sel = scores + router_b.astype(jnp.float32)
    grp_score = lax.top_k(sel.reshape(n_tok, N_GROUPS, N_EXPERTS // N_GROUPS), 2)[0].sum(-1)
    _, gidx = lax.top_k(grp_score, TOPK_GROUPS)
    gmask = jnp.any(gidx[:, :, None] == jnp.arange(N_GROUPS)[None, None, :], axis=1)
    sel = jnp.where(jnp.repeat(gmask, N_EXPERTS // N_GROUPS, axis=1), sel, -jnp.inf)
    _, eidx = lax.top_k(sel, TOP_K)
    gw = jnp.take_along_axis(scores, eidx, axis=1)
    gw = gw / jnp.sum(gw, axis=-1, keepdims=True) * ROUTED_SCALE

    n_asg = n_tok * TOP_K
    flat_e = eidx.reshape(-1)
    flat_t = jnp.repeat(jnp.arange(n_tok, dtype=jnp.int32), TOP_K)
    flat_w = gw.reshape(-1)
    order = jnp.argsort(flat_e)
    se = flat_e[order]
    counts = jnp.bincount(flat_e, length=N_EXPERTS)
    padded = (counts + MOE_BLOCK - 1) // MOE_BLOCK * MOE_BLOCK
    pend = jnp.cumsum(padded)
    pstart = pend - padded
    ustart = jnp.cumsum(counts) - counts
    dest = pstart[se] + jnp.arange(n_asg, dtype=jnp.int32) - ustart[se]
    n_blocks = (n_asg + N_EXPERTS * (MOE_BLOCK - 1)) // MOE_BLOCK + 1
    cap = n_blocks * MOE_BLOCK
    slot_tok = jnp.full((cap,), n_tok, jnp.int32).at[dest].set(flat_t[order])
    slot_w = jnp.zeros((cap,), jnp.float32).at[dest].set(flat_w[order])
    block_start = jnp.arange(n_blocks, dtype=pend.dtype) * MOE_BLOCK
    block_e = jnp.minimum(jnp.searchsorted(pend, block_start, side='right'), N_EXPERTS - 1)

    t_pad = jnp.concatenate([t, jnp.zeros((1, d), t.dtype)], axis=0)

    def expert_block(args):
        tok, e = args
        xb = t_pad[tok]
        hb = jax.nn.silu(xb @ w_gate[e]) * (xb @ w_up[e])
        return hb @ w_down[e]

    yb = lax.map(expert_block, (slot_tok.reshape(n_blocks, MOE_BLOCK), block_e))
    yb = yb.reshape(cap, d) * slot_w[:, None].astype(yb.dtype)
    routed = jax.ops.segment_sum(yb, slot_tok, num_segments=n_tok + 1)[:n_tok]
    shared = (jax.nn.silu(t @ s_gate) * (t @ s_up)) @ s_down
    return routed + shared


def setup_inputs(seed: int = 0) -> dict:
    key = jax.random.key(seed)
    ks = iter(jax.random.split(key, 32))
    f32 = jnp.float32

    def nrm(shape, scale):
        return jax.random.normal(next(ks), shape, f32) * scale

    d = D_MODEL
    n_even = (DEPTH + 1) // 2
    n_odd = DEPTH // 2
    a_in = 3 * CONV_DIM + (ATT_HEADS + 2 * ATT_KV_HEADS) * HEAD_DIM
    a_mix = CONV_DIM + ATT_HEADS * HEAD_DIM
    m_mix = MLA_HEADS * V_DIM
    return {
        'x': nrm((BATCH, SEQ, d), 1.0),
        'c': nrm((BATCH, d), 1.0),
        'ctx': nrm((BATCH, CTX_LEN, d), 1.0),
        'c_ctx': nrm((d,), 1.0),
        'w_ada': nrm((DEPTH, d, 6 * d), 0.5 * d ** -0.5),
        'b_ada': nrm((DEPTH, 6 * d), 0.02),
        'ln_g': 1.0 + nrm((DEPTH, 2, d), 0.02),
        'ln_b': nrm((DEPTH, 2, d), 0.02),
        'a_w_in': nrm((n_even, d, a_in), d ** -0.5),
        'a_conv_w': nrm((n_even, CONV_K, CONV_DIM), CONV_K ** -0.5),
        'a_q_gain': 1.0 + nrm((n_even, HEAD_DIM), 0.02),
        'a_k_gain': 1.0 + nrm((n_even, HEAD_DIM), 0.02),
        'a_w_out': nrm((n_even, a_mix, d), DN_BETA * a_mix ** -0.5),
        'm_w_down': nrm((n_odd, d, Q_LORA + KV_LORA + QK_ROPE), d ** -0.5),
        'm_q_gain': 1.0 + nrm((n_odd, Q_LORA), 0.02),
        'm_kv_gain': 1.0 + nrm((n_odd, KV_LORA), 0.02),
        'm_w_uq': nrm((n_odd, Q_LORA, MLA_HEADS * (QK_NOPE + QK_ROPE)), Q_LORA ** -0.5),
        'm_w_ukv': nrm((n_odd, KV_LORA, MLA_HEADS * (QK_NOPE + V_DIM)), KV_LORA ** -0.5),
        'm_w_out': nrm((n_odd, m_mix, d), DN_BETA * m_mix ** -0.5),
        'router_w': nrm((DEPTH, d, N_EXPERTS), d ** -0.5),
        'router_b': nrm((DEPTH, N_EXPERTS), 0.01),
        'e_w_gate': nrm((DEPTH, N_EXPERTS, d, EXPERT_FF), d ** -0.5),
        'e_w_up': nrm((DEPTH, N_EXPERTS, d, EXPERT_FF), d ** -0.5),
        'e_w_down': nrm((DEPTH, N_EXPERTS, EXPERT_FF, d), DN_BETA * EXPERT_FF ** -0.5),
        's_w_gate': nrm((DEPTH, d, SHARED_FF), d ** -0.5),
        's_w_up': nrm((DEPTH, d, SHARED_FF), d ** -0.5),
        's_w_down': nrm((DEPTH, SHARED_FF, d), DN_BETA * SHARED_FF ** -0.5),
    }


def reference(x, c, ctx, c_ctx, w_ada, b_ada, ln_g, ln_b, a_w_in, a_conv_w, a_q_gain, a_k_gain, a_w_out,
              m_w_down, m_q_gain, m_kv_gain, m_w_uq, m_w_ukv, m_w_out, router_w, router_b,
              e_w_gate, e_w_up, e_w_down, s_w_gate, s_w_up, s_w_down):
    h = ctx
    n_lat = x.shape[0] * x.shape[1]
    silu_c = jax.nn.silu(c)
    silu_cc = jax.nn.silu(c_ctx)
    for i in range(DEPTH):
        last = i == DEPTH - 1
        j = i // 2
        mod = (silu_c @ w_ada[i] + b_ada[i])[:, None, :]
        mod_c = silu_cc @ w_ada[i] + b_ada[i]
        sh_m, sc_m, g_m, sh_f, sc_f, g_f = jnp.split(mod, 6, axis=-1)
        shc_m, scc_m, gc_m, shc_f, scc_f, gc_f = jnp.split(mod_c, 6, axis=-1)

        u = x * (1 + sc_m) + sh_m
        uc = h * (1 + scc_m) + shc_m
        if i % 2 == 0:
            y, yc = conv_attn_mixer(u, uc, a_w_in[j], a_conv_w[j], a_q_gain[j], a_k_gain[j], a_w_out[j], not last)
        else:
            y, yc = mla_mixer(u, uc, m_w_down[j], m_q_gain[j], m_kv_gain[j], m_w_uq[j], m_w_ukv[j], m_w_out[j], not last)
        x = layer_norm(DN_ALPHA * x + g_m * y, ln_g[i, 0], ln_b[i, 0])

        v = x * (1 + sc_f) + sh_f
        tok = v.reshape(-1, v.shape[-1])
        if not last:
            h = layer_norm(DN_ALPHA * h + gc_m * yc, ln_g[i, 0], ln_b[i, 0])
            vc = h * (1 + scc_f) + shc_f
            tok = jnp.concatenate([tok, vc.reshape(-1, vc.shape[-1])], axis=0)
        f = moe_ffn(tok, router_w[i], router_b[i], e_w_gate[i], e_w_up[i], e_w_down[i],
                    s_w_gate[i], s_w_up[i], s_w_down[i])
        x = layer_norm(DN_ALPHA * x + g_f * f[:n_lat].reshape(x.shape), ln_g[i, 1], ln_b[i, 1])
        if not last:
            h = layer_norm(DN_ALPHA * h + gc_f * f[n_lat:].reshape(h.shape), ln_g[i, 1], ln_b[i, 1])
    return x
```

```python
import contextlib
import math
import re
import numpy as np
import ml_dtypes
import concourse.bass as bass
import concourse.mybir as mybir
from concourse.bass_utils import run_bass_kernel_spmd

F32 = mybir.dt.float32
BF16 = mybir.dt.bfloat16
AF = mybir.ActivationFunctionType
ALU = mybir.AluOpType
AX = mybir.AxisListType

D = 2048
KD = 16
DEPTH = 2
LN_EPS = 1e-5
RMS_EPS = 1e-6
DN_ALPHA = (2 * DEPTH) ** 0.25
GRID_W = 64
ROPE_THETA = 10000.0
NG = 8
TOPG = 4
TOPK = 6
ROUTED_SCALE = 2.5
G8 = [list(range(8))]
G2 = [[0, 1], [2, 3], [4, 5], [6, 7]]


class Cfg:
    def __init__(self, SEQ=4096, CTX=256, NE=64, FF=512):
        self.SEQ, self.CTX, self.NE, self.FF = SEQ, CTX, NE, FF


class Sched:
    def __init__(self, nc, stack):
        self.nc = nc
        self.stack = stack
        self.eng = {'pe': nc.tensor, 'act': nc.scalar, 'dve': nc.vector, 'pool': nc.gpsimd, 'sp': nc.sync}
        self.esem = {}
        self.cnt = {}
        self.sems = {}
        for k in ['pe', 'act', 'dve', 'pool']:
            s = stack.enter_context(nc.semaphore('es_' + k))
            self.esem[k] = s
            self.cnt[id(s)] = 0
            self.sems[id(s)] = (s, k)
        self.waited = {}
        self.w = {}
        self.r = {}
        self.dsem = {}
        self.free = []
        self.nalloc = 0
        self.persist = set()
        self.ninst = 0

    def _deps(self, reads, writes, join):
        d = {}
        for k in reads:
            d.update(self.w.get(k, {}))
        for k in writes:
            if not join:
                for sid, v in self.w.get(k, {}).items():
                    d[sid] = max(d.get(sid, 0), v)
            for sid, v in self.r.get(k, {}).items():
                d[sid] = max(d.get(sid, 0), v)
        for k in reads:
            for sid, v in self.w.get(k, {}).items():
                d[sid] = max(d.get(sid, 0), v)
        return d

    def _wait(self, engine, deps):
        e = self.eng[engine]
        for sid, val in deps.items():
            sem, src = self.sems[sid]
            if src == engine and engine == 'pe':
                continue
            if src is None:
                val = self.cnt[sid]
            if self.waited.get((engine, sid), 0) >= val:
                continue
            e.wait_ge(sem, val)
            self.waited[(engine, sid)] = val
            self.ninst += 1

    def _record(self, sid, val, reads, writes, join):
        for k in reads:
            self.r.setdefault(k, {})[sid] = val
        for k in writes:
            if join:
                self.w.setdefault(k, {})[sid] = val
            else:
                self.w[k] = {sid: val}
                self.r[k] = {}

    def op(self, engine, fn, reads=(), writes=()):
        self._wait(engine, self._deps(reads, writes, False))
        inst = fn(self.eng[engine])
        sem = self.esem[engine]
        self.cnt[id(sem)] += 1
        inst.then_inc(sem, 1)
        self._record(id(sem), self.cnt[id(sem)], reads, writes, False)
        self.ninst += 1
        return inst

    def _dsem(self, key):
        if key not in self.dsem:
            if self.free:
                s = self.free.pop()
            else:
                s = self.stack.enter_context(self.nc.semaphore('ds%d' % self.nalloc))
                self.nalloc += 1
                self.cnt[id(s)] = 0
                self.sems[id(s)] = (s, None)
            self.dsem[key] = s
        return self.dsem[key]

    def dma(self, queue, out, in_, reads=(), writes=(), join=False, sem=None, **kw):
        import os
        if writes and str(writes[0]) in os.environ.get('K_SKIP', '').split(','):
            return None
        s = self._dsem(sem or writes[0])
        self._wait(queue, self._deps(reads, writes, join))
        inst = self.eng[queue].dma_start(out=out, in_=in_, **kw)
        self.cnt[id(s)] += 16
        inst.then_inc(s, 16)
        self._record(id(s), self.cnt[id(s)], reads, writes, join)
        self.ninst += 1
        return inst

    def cc(self, kind, groups, in_ap, out_ap, reads, writes):
        s = self._dsem(writes[0])
        self._wait('pool', self._deps(reads, writes, False))
        op = ALU.bypass if kind == "AllGather" else ALU.add
        inst = self.nc.gpsimd.collective_compute(kind, op, replica_groups=groups, ins=[in_ap], outs=[out_ap])
        self.cnt[id(s)] += 1
        inst.then_inc(s, 1)
        self._record(id(s), self.cnt[id(s)], reads, writes, False)
        self.ninst += 1

    def barrier(self):
        keep = {id(self.dsem[k]) for k in self.persist if k in self.dsem}
        toks = {sid: c for sid, c in self.cnt.items() if c > 0 and sid not in keep}
        for eng in ['pe', 'act', 'dve', 'pool', 'sp']:
            self._wait(eng, dict(toks))
        self.w = {k: v for k, v in self.w.items() if k in self.persist}
        self.r = {}
        for k in list(self.dsem):
            if k not in self.persist:
                self.free.append(self.dsem.pop(k))


def build(cfg):
    nc = bass.Bass("TRN2", target_bir_lowering=False)
    SL = cfg.SEQ // 2
    CL = cfg.CTX // 2
    T = SL + CL
    NE = cfg.NE
    EPC = NE // 8
    GS = NE // NG
    FF = cfg.FF
    FFC = FF // 128
    NLT = SL // 512
    NST = T // 128
    own_tiles = [(i * 512, 512, False) for i in range(NLT)] + [(SL, CL, True)]
    lat_tiles = own_tiles[:NLT]

    def inp(name, shape, dt=F32):
        return nc.dram_tensor(name, list(shape), dt, kind="ExternalInput").ap()

    def scr(name, shape, dt=F32):
        return nc.dram_tensor(name, list(shape), dt, kind="Internal").ap()

    I = {}
    for name, shape in [
        ('x_own', [T, D]), ('halo_x', [64, D]), ('halo_mask', [128, 64]), ('cT_own', [128, 2, 8]),
        ('w_ada_k', [2, 256, 6 * D]), ('b_ada', [2, 6 * D]), ('sel_own', [6, 128]), ('sel_ctx', [6, 128]),
        ('ln_g', [4, D]), ('ln_b', [4, D]),
        ('a_w_in_s', [256, 4608]), ('a_w_out_s', [256, D]), ('m_w_down_s', [256, 1088]), ('m_w_uq_s', [64, 3072]),
        ('m_w_ukv_s', [64, 4096]), ('m_w_out_s', [256, D]), ('s_gate_s', [512, FF]), ('s_up_s', [512, FF]),
        ('s_down_s', [2 * FF // 8, D]),
        ('conv_w_t', [128, 3, 8]), ('a_q_gain', [128, 1]), ('a_k_gain', [128, 1]), ('m_q_gain_t', [128, 4]),
        ('m_kv_gain_t', [128, 4]), ('router_w_t', [2, 128, KD, NE]), ('router_b', [2, NE]),
        ('e_gate', [2, EPC, D, FF]), ('e_up', [2, EPC, D, FF]), ('e_down', [2, EPC, FF, D]),
        ('cos0', [128, SL]), ('sin0', [128, SL]), ('cos1', [64, SL]), ('sin1', [64, SL]),
        ('r128t', [128, 128]), ('r64t', [64, 64]), ('ident', [128, 128]), ('sel_exp', [128, EPC, NE]),
    ]:
        I[name] = inp(name, shape)
    out = nc.dram_tensor("out", [SL, D], F32, kind="ExternalOutput").ap()

    modpart = scr('modpart', [16, 6 * D])
    modsum = scr('modsum', [16, 6 * D])
    Wf = {n: scr(n + '_f', [8 * I[n + '_s'].shape[0], I[n + '_s'].shape[1]]) for n in
          ['a_w_in', 'a_w_out', 'm_w_down', 'm_w_uq', 'm_w_ukv', 'm_w_out', 's_gate', 's_up', 's_down']}
    Wsi = {n: scr(n + '_si', list(I[n + '_s'].shape)) for n in Wf}
    Wb = {
        'a_w_in': scr('a_w_in_b', [128, KD, 4608], BF16), 'a_w_out': scr('a_w_out_b', [128, KD, D], BF16),
        'm_w_down': scr('m_w_down_b', [128, KD, 1088], BF16), 'm_w_uq': scr('m_w_uq_b', [128, 4, 3072], BF16),
        'm_w_ukv': scr('m_w_ukv_b', [128, 4, 4096], BF16), 'm_w_out': scr('m_w_out_b', [128, KD, D], BF16),
        's_gate': scr('s_gate_b', [2, 128, KD, FF], BF16), 's_up': scr('s_up_b', [2, 128, KD, FF], BF16),
        's_down': scr('s_down_b', [2, 128, FFC, D], BF16),
        'e_gate': scr('e_gate_b', [2, EPC, 128, KD, FF], BF16), 'e_up': scr('e_up_b', [2, EPC, 128, KD, FF], BF16),
        'e_down': scr('e_down_b', [2, EPC, 128, FFC, D], BF16),
    }
    GB = scr('GB', [128, 8, T], BF16)
    QT = scr('QT', [128, 8, T], BF16)
    KT_own = scr('KT_own', [256, T], BF16)
    V_own = scr('V_own', [T, 256], BF16)
    KT_pair = scr('KT_pair', [512, T], BF16)
    V_pair = scr('V_pair', [2 * T, 256], BF16)
    X1 = scr('X1', [T, D])
    X2 = scr('X2', [T, D])
    X3 = scr('X3', [T, D])
    NT_ = len(own_tiles)
    VTo = [scr('VTo%d' % j, [D, own_tiles[j][1]], BF16) for j in range(NT_)]
    VTa = [scr('VTa%d' % j, [8 * D, own_tiles[j][1]], BF16) for j in range(NT_)]
    Pp = [scr('Pp%d' % j, [8 * own_tiles[j][1], D]) for j in range(NT_)]
    Fo = [scr('Fo%d' % j, [own_tiles[j][1], D]) for j in range(NT_)]
    G_own = scr('G_own', [T, NE])
    G_all = scr('G_all', [8 * T, NE])
    Fsh = scr('Fsh', [T, D])
    KVc_own = [scr('KVc_own%d' % i, [256, T], BF16) for i in range(2)]
    KVc_pair = [scr('KVc_pair%d' % i, [512, T], BF16) for i in range(2)]
    KR_own = scr('KR_own', [64, T], BF16)
    KR_pair = scr('KR_pair', [128, T], BF16)
    CQ = scr('CQ', [128, 4, SL], BF16)
    AT1 = scr('AT1', [128, 16, T], BF16)
    AT0 = scr('AT0', [128, 16, T], BF16)
    ZT0 = scr('ZT0', [128, 8, T + 4], BF16)

    with contextlib.ExitStack() as st:
        S = Sched(nc, st)
        S.persist = set()
        ps = [st.enter_context(nc.psum_tensor("ps%d" % i, [128, 512], F32)) for i in range(8)]

        def PS(i):
            return ('ps', i)

        def consts(ph):
            def Tl(name, shape, dt=F32):
                return ph.enter_context(nc.sbuf_tensor('P0_' + name, list(shape), dt))
            return Tl

        with contextlib.ExitStack() as ph:
            Tl = consts(ph)
            for n in Wf:
                S.dma('sp', Wsi[n][:, :], I[n + '_s'][:, :], writes=[n + '_si'])
                S.cc("AllGather", G8, Wsi[n][:, :], Wf[n][:, :], reads=[n + '_si'], writes=[n + '_f'])
            for n in ['a_w_in', 'a_w_out', 'm_w_down', 'm_w_out']:
                src = Wf[n].rearrange("(k p) c -> p k c", p=128)
                for k in range(KD):
                    S.dma('pool', Wb[n][:, k, :], src[:, k, :], reads=[n + '_f'], writes=[n + '_b'], join=True)
            for n in ['m_w_uq', 'm_w_ukv']:
                src = Wf[n].rearrange("(k p) c -> p k c", p=128)
                for k in range(4):
                    S.dma('pool', Wb[n][:, k, :], src[:, k, :], reads=[n + '_f'], writes=[n + '_b'], join=True)
            for n in ['s_gate', 's_up']:
                src = Wf[n].rearrange("(l k p) c -> l p k c", l=2, p=128)
                for l in range(2):
                    S.dma('pool', Wb[n][l], src[l], reads=[n + '_f'], writes=[n + '_b'], join=True)
            src = Wf['s_down'].rearrange("(l k p) c -> l p k c", l=2, p=128)
            for l in range(2):
                S.dma('pool', Wb['s_down'][l], src[l], reads=['s_down_f'], writes=['s_down_b'], join=True)
            for l in range(2):
                for e in range(EPC):
                    S.dma('pool', Wb['e_gate'][l, e], I['e_gate'][l, e].rearrange("(k p) c -> p k c", p=128),
                          writes=['e_gate_b'], join=True)
                    S.dma('pool', Wb['e_up'][l, e], I['e_up'][l, e].rearrange("(k p) c -> p k c", p=128),
                          writes=['e_up_b'], join=True)
                    S.dma('pool', Wb['e_down'][l, e], I['e_down'][l, e].rearrange("(k p) c -> p k c", p=128),
                          writes=['e_down_b'], join=True)
            cT = Tl('cT', [128, 2, 8])
            scT = Tl('scT', [128, 2, 8])
            S.dma('sp', cT[:], I['cT_own'][:, :, :], writes=['cT'])
            S.op('act', lambda e: e.activation(out=scT[:], in_=cT[:], func=AF.Silu), reads=['cT'], writes=['scT'])
            wa = [Tl('wa%d' % i, [128, 2, 2048]) for i in range(2)]
            mo = [Tl('mo%d' % i, [8, 2048]) for i in range(2)]
            it = 0
            for l in range(2):
                for cg in range(6):
                    b = it % 2
                    S.dma('sp', wa[b][:], I['w_ada_k'][l, :, cg * 2048:(cg + 1) * 2048].rearrange("(k p) c -> p k c", p=128),
                          writes=['wa%d' % b])
                    for n in range(4):
                        pb = (it * 4 + n) % 8
                        for k in range(2):
                            S.op('pe', lambda e: e.matmul(ps[pb][0:8, :], scT[:, k, :], wa[b][:, k, n * 512:(n + 1) * 512],
                                                          start=(k == 0), stop=(k == 1)),
                                 reads=['scT', 'wa%d' % b], writes=[PS(pb)])
                        S.op('act', lambda e: e.copy(out=mo[b][:, n * 512:(n + 1) * 512], in_=ps[pb][0:8, :]),
                             reads=[PS(pb)], writes=['mo%d' % b])
                    S.dma('sp', modpart[l * 8:(l + 1) * 8, cg * 2048:(cg + 1) * 2048], mo[b][:], reads=['mo%d' % b],
                          writes=['modpart'], join=True)
                    it += 1
            S.cc("AllReduce", G8, modpart[:, :], modsum[:, :], reads=['modpart'], writes=['modsum'])
            S.barrier()

        phase_no = [0]

        def mk(ph):
            phase_no[0] += 1
            pfx = 'P%d_' % phase_no[0]

            def Tl(name, shape, dt=F32):
                return ph.enter_context(nc.sbuf_tensor(pfx + name, list(shape), dt))
            return Tl

        def nm(t):
            n = t.tensor.name if hasattr(t, 'tensor') else t.name
            return re.sub(r'^P\d+_', '', n)

        def load_consts(Tl):
            c = {}
            c['ident'] = Tl('ident', [128, 128])
            S.dma('sp', c['ident'][:], I['ident'][:, :], writes=['ident'])
            c['identb'] = Tl('identb', [128, 128], BF16)
            S.op('dve', lambda e: e.tensor_copy(out=c['identb'][:], in_=c['ident'][:]), reads=['ident'], writes=['identb'])
            c['sel_own'] = Tl('sel_own', [6, 128])
            c['sel_ctx'] = Tl('sel_ctx', [6, 128])
            S.dma('sp', c['sel_own'][:], I['sel_own'][:, :], writes=['sel_own'])
            S.dma('sp', c['sel_ctx'][:], I['sel_ctx'][:, :], writes=['sel_ctx'])
            c['mod6'] = Tl('mod6', [6, D])
            return c

        def bcast_param(c, dst, l, which, ctx, scale=1.0, bias=0.0, p0=0, p1=128):
            seg = slice(which * D, (which + 1) * D)
            S.dma('sp', c['mod6'][0:5, :], modsum[l * 8:l * 8 + 5, seg], reads=['modsum'], writes=['mod6'])
            S.dma('sp', c['mod6'][5:6, :], I['b_ada'][l:l + 1, seg], writes=['mod6'], join=True)
            sel = c['sel_ctx'] if ctx else c['sel_own']
            for n in range(4):
                pb = n
                S.op('pe', lambda e: e.matmul(ps[pb][:], sel[:], c['mod6'][:, n * 512:(n + 1) * 512], start=True, stop=True),
                     reads=['mod6', 'sel_own', 'sel_ctx'], writes=[PS(pb)])
                S.op('act', lambda e: e.activation(out=dst[p0:p1, n * 512:(n + 1) * 512], in_=ps[pb][p0:p1, :], func=AF.Identity,
                                                   scale=float(scale), bias=float(bias)),
                     reads=[PS(pb)], writes=[nm(dst)])

        def bcast_row(dst, src_ap, key):
            n = dst.shape[1]
            S.dma('sp', dst[:], bass.AP(src_ap.tensor, src_ap.offset, [[0, 128], [1, n]]), writes=[key])

        def transpose_to(c, src, key_src, npart, dstT, key_dst, col0, bf, banks=(0, 1, 2, 3), cast_dst=None, key_cast=None):
            idn = c['identb'] if bf else c['ident']
            for kq in range(4):
                pb = banks[kq % len(banks)]
                pv = ps[pb][:].bitcast(BF16) if bf else ps[pb][:]
                for kk in range(4):
                    k = kq * 4 + kk
                    S.op('pe', lambda e: e.transpose(out=pv[:, kk * 128:kk * 128 + npart], in_=src[0:npart, k * 128:(k + 1) * 128],
                                                     identity=idn[0:npart, 0:npart]),
                         reads=[key_src, 'identb', 'ident'], writes=[PS(pb)])
                srcv = pv[:, 0:512].rearrange("p (k t) -> p k t", k=4)[:, :, 0:npart]
                dv = dstT[:, kq * 4:(kq + 1) * 4, col0:col0 + npart]
                if kq % 2 == 0:
                    S.op('act', lambda e: e.copy(out=dv, in_=srcv), reads=[PS(pb)], writes=[key_dst])
                else:
                    S.op('dve', lambda e: e.tensor_copy(out=dv, in_=srcv), reads=[PS(pb)], writes=[key_dst])
                if cast_dst is not None:
                    cv = cast_dst[:, kq * 4:(kq + 1) * 4, col0:col0 + npart]
                    S.op('pool', lambda e: e.tensor_copy(out=cv, in_=dv), reads=[key_dst], writes=[key_cast])

        def modulate(xs, key_x, segs, tmp, outb):
            for (p0, p1, A, Bv) in segs:
                S.op('dve', lambda e: e.tensor_tensor(out=tmp[p0:p1, :], in0=xs[p0:p1, :], in1=A[p0:p1, :], op=ALU.mult),
                     reads=[key_x, nm(A)], writes=[nm(tmp)])
                S.op('pool', lambda e: e.tensor_tensor(out=outb[p0:p1, :], in0=tmp[p0:p1, :], in1=Bv[p0:p1, :], op=ALU.add),
                     reads=[nm(tmp), nm(Bv)], writes=[nm(outb)])

        def layer_norm(r, key_r, st_t, junk, lng, lnb, x1):
            ks = nm(st_t)
            S.op('act', lambda e: e.activation(out=junk[:], in_=r, func=AF.Identity, accum_out=st_t[:, 0:1]),
                 reads=[key_r], writes=[nm(junk), ks])
            S.op('act', lambda e: e.activation(out=junk[:], in_=r, func=AF.Square, accum_out=st_t[:, 1:2]),
                 reads=[key_r], writes=[nm(junk), ks])
            S.op('dve', lambda e: e.tensor_scalar(out=st_t[:, 2:3], in0=st_t[:, 0:1], scalar1=1.0 / D, scalar2=None, op0=ALU.mult),
                 reads=[ks], writes=[ks])
            S.op('dve', lambda e: e.tensor_tensor(out=st_t[:, 3:4], in0=st_t[:, 2:3], in1=st_t[:, 2:3], op=ALU.mult),
                 reads=[ks], writes=[ks])
            S.op('dve', lambda e: e.scalar_tensor_tensor(out=st_t[:, 4:5], in0=st_t[:, 1:2], scalar=1.0 / D, in1=st_t[:, 3:4],
                                                         op0=ALU.mult, op1=ALU.subtract), reads=[ks], writes=[ks])
            S.op('act', lambda e: e.activation(out=st_t[:, 5:6], in_=st_t[:, 4:5], func=AF.Sqrt, bias=float(LN_EPS / DN_ALPHA ** 2), scale=1.0),
                 reads=[ks], writes=[ks])
            S.op('dve', lambda e: e.reciprocal(out=st_t[:, 6:7], in_=st_t[:, 5:6]), reads=[ks], writes=[ks])
            S.op('dve', lambda e: e.scalar_tensor_tensor(out=st_t[:, 7:8], in0=st_t[:, 2:3], scalar=-1.0, in1=st_t[:, 6:7],
                                                         op0=ALU.mult, op1=ALU.mult), reads=[ks], writes=[ks])
            S.op('act', lambda e: e.activation(out=r, in_=r, func=AF.Identity, scale=st_t[:, 6:7], bias=st_t[:, 7:8]),
                 reads=[key_r, ks], writes=[key_r])
            S.op('pool', lambda e: e.tensor_tensor(out=junk[:], in0=r, in1=lng[:], op=ALU.mult), reads=[key_r, nm(lng)], writes=[nm(junk)])
            S.op('pool', lambda e: e.tensor_tensor(out=x1[:], in0=junk[:], in1=lnb[:], op=ALU.add), reads=[nm(junk), nm(lnb)], writes=[nm(x1)])

        def attn_tile(score_ops, v_ops, ntok, scale, pT, ones_bf, rec, out_ap, key_out, extra_reads, par=0):
            nk = len(score_ops)
            ob, db = (4, 5) if par % 2 == 0 else (6, 7)

            def score(kc):
                sb = kc % 4
                so = score_ops[kc]
                for i, (lt, rh) in enumerate(so):
                    S.op('pe', lambda e: e.matmul(ps[sb][:, 0:ntok], lt, rh, start=(i == 0), stop=(i == len(so) - 1)),
                         reads=extra_reads, writes=[PS(sb)])

            score(0)
            if nk > 1:
                score(1)
            for kc in range(nk):
                sb = kc % 4
                if kc + 2 < nk:
                    score(kc + 2)
                pt = pT[:, kc % 3, 0:ntok]
                S.op('act', lambda e: e.activation(out=pt, in_=ps[sb][:, 0:ntok], func=AF.Exp, scale=float(scale)),
                     reads=[PS(sb)], writes=[('pT', kc % 3)])
                S.op('pe', lambda e: e.matmul(ps[ob][:, 0:ntok], v_ops[kc], pt, start=(kc == 0), stop=(kc == nk - 1)),
                     reads=[('pT', kc % 3)] + extra_reads, writes=[PS(ob)])
                S.op('pe', lambda e: e.matmul(ps[db][:, 0:ntok], ones_bf[:], pt, start=(kc == 0), stop=(kc == nk - 1)),
                     reads=[('pT', kc % 3), 'ones_bf'], writes=[PS(db)])
            np_ = out_ap.shape[0]
            S.op('dve', lambda e: e.reciprocal(out=rec[0:np_, 0:ntok], in_=ps[db][0:np_, 0:ntok]), reads=[PS(db)], writes=['rec'])
            S.op('dve', lambda e: e.tensor_tensor(out=out_ap, in0=ps[ob][0:np_, 0:ntok], in1=rec[0:np_, 0:ntok], op=ALU.mult),
                 reads=[PS(ob), 'rec'], writes=[key_out])

        def rope(src32, key_src, npart, ntok, rT, cosT, sinT, t1, out_ap, key_out, pb):
            S.op('pe', lambda e: e.matmul(ps[pb][0:npart, 0:ntok], rT[0:npart, 0:npart], src32, start=True, stop=True),
                 reads=[key_src, 'rT'], writes=[PS(pb)])
            S.op('pool', lambda e: e.tensor_tensor(out=t1[0:npart, 0, 0:ntok], in0=src32, in1=cosT, op=ALU.mult),
                 reads=[key_src, 'cos'], writes=[('t1', 0)])
            S.op('dve', lambda e: e.tensor_tensor(out=t1[0:npart, 1, 0:ntok], in0=ps[pb][0:npart, 0:ntok], in1=sinT, op=ALU.mult),
                 reads=[PS(pb), 'sin'], writes=[('t1', 1)])
            S.op('pool', lambda e: e.tensor_tensor(out=out_ap, in0=t1[0:npart, 0, 0:ntok], in1=t1[0:npart, 1, 0:ntok], op=ALU.add),
                 reads=[('t1', 0), ('t1', 1)], writes=[key_out])

        def ffn_tile(v, kv, ntok, w, mode, acc, gsc, key_g, hm, sg):
            nsub = ntok // 128
            (wg_, wu_, wd_, kg, ku, kd_, gcol) = w
            for j in range(FFC):
                bg = (j % 2) * 2
                bu = bg + 1
                for k in range(KD):
                    S.op('pe', lambda e: e.matmul(ps[bg][:, 0:ntok], wg_[:, k, j * 128:(j + 1) * 128], v[:, k, 0:ntok],
                                                  start=(k == 0), stop=(k == KD - 1)), reads=[kg, kv], writes=[PS(bg)])
                for k in range(KD):
                    S.op('pe', lambda e: e.matmul(ps[bu][:, 0:ntok], wu_[:, k, j * 128:(j + 1) * 128], v[:, k, 0:ntok],
                                                  start=(k == 0), stop=(k == KD - 1)), reads=[ku, kv], writes=[PS(bu)])
                S.op('act', lambda e: e.activation(out=sg[:, j % 2, 0:ntok], in_=ps[bg][:, 0:ntok], func=AF.Silu),
                     reads=[PS(bg)], writes=[('sg', j % 2)])
                S.op('dve', lambda e: e.tensor_tensor(out=hm[:, j, 0:ntok], in0=ps[bu][:, 0:ntok], in1=sg[:, j % 2, 0:ntok],
                                                      op=ALU.mult), reads=[PS(bu), ('sg', j % 2)], writes=[('hm', j)])
            q = 0
            for s in range(nsub):
                for n in range(4):
                    pb = 4 + q % 4
                    q += 1
                    for j in range(FFC):
                        S.op('pe', lambda e: e.matmul(ps[pb][:], hm[:, j, s * 128:(s + 1) * 128], wd_[:, j, n * 512:(n + 1) * 512],
                                                      start=(j == 0), stop=(j == FFC - 1)), reads=[('hm', j), kd_], writes=[PS(pb)])
                    dst = acc[:, s, n * 512:(n + 1) * 512]
                    ka = ('acc', s, n)
                    if mode == 'copy':
                        S.op('dve', lambda e: e.tensor_copy(out=dst, in_=ps[pb][:]), reads=[PS(pb)], writes=[ka])
                    elif mode == 'gfirst':
                        S.op('dve', lambda e: e.tensor_scalar(out=dst, in0=ps[pb][:], scalar1=gsc[:, s, gcol:gcol + 1], scalar2=None,
                                                              op0=ALU.mult), reads=[PS(pb), key_g], writes=[ka])
                    else:
                        S.op('dve', lambda e: e.scalar_tensor_tensor(out=dst, in0=ps[pb][:], scalar=gsc[:, s, gcol:gcol + 1], in1=dst,
                                                                     op0=ALU.mult, op1=ALU.add), reads=[PS(pb), key_g, ka], writes=[ka])

        def phase_l0_proj():
            with contextlib.ExitStack() as ph:
                Tl = mk(ph)
                c = load_consts(Tl)
                Al, Bl, Ac, Bc = Tl('Al', [128, D]), Tl('Bl', [128, D]), Tl('Ac', [128, D]), Tl('Bc', [128, D])
                bcast_param(c, Al, 0, 1, False, bias=1.0)
                bcast_param(c, Bl, 0, 0, False)
                bcast_param(c, Ac, 0, 1, True, bias=1.0)
                bcast_param(c, Bc, 0, 0, True)
                xs = [Tl('xs%d' % i, [128, D]) for i in range(2)]
                tmp = Tl('tmp', [128, D])
                tmpb = Tl('tmpb', [128, D], BF16)
                uT = Tl('uT', [128, KD, 512], BF16)
                wg = [Tl('wg%d' % i, [128, KD, 512], BF16) for i in range(2)]
                gcT = Tl('gcT', [128, 8, 512], BF16)
                zt = Tl('zt', [128, 4, 512], BF16)
                gbo = Tl('gbo', [128, 4, 512], BF16)
                qko = Tl('qko', [128, 10, 512], BF16)
                vsb = Tl('vsb', [128, 4, 256], BF16)
                sq = Tl('sq', [128, 512])
                sd = Tl('sd', [128, 512])
                rstd = Tl('rstd', [128, 512])
                qn = Tl('qn', [128, 512])
                t1 = Tl('t1', [128, 2, 512])
                cosT, sinT = Tl('cosT', [128, 512]), Tl('sinT', [128, 512])
                rT = Tl('rT', [128, 128])
                onesm = Tl('onesm', [128, 128])
                gq, gk = Tl('gq', [128, 1]), Tl('gk', [128, 1])
                hmask = Tl('hmask', [128, 64])
                S.dma('sp', rT[:], I['r128t'][:, :], writes=['rT'])
                S.dma('sp', gq[:], I['a_q_gain'][:, :], writes=['gq'])
                S.dma('sp', gk[:], I['a_k_gain'][:, :], writes=['gk'])
                S.dma('sp', hmask[:], I['halo_mask'][:, :], writes=['hmask'])
                S.op('dve', lambda e: e.memset(onesm[:], 1.0 / 128), writes=['onesm'])
                ZT = ZT0
                wcount = [0]
                tiles = [(0, 64, 'halo')] + [(t0, n, 'ctx' if cx else 'lat') for (t0, n, cx) in own_tiles]
                import os
                if os.environ.get('K_TILES'):
                    tiles = [tiles[int(i)] for i in os.environ['K_TILES'].split(',')]
                xi = 0
                for (tok0, ntok, kind) in tiles:
                    nsub = max(1, ntok // 128)
                    for s in range(nsub):
                        xb = xs[xi % 2]
                        xi += 1
                        if kind == 'halo':
                            S.dma('sp', xb[0:64, :], I['halo_x'][:, :], writes=[nm(xb)])
                            modulate(xb, nm(xb), [(0, 32, Al, Bl), (32, 64, Ac, Bc)], tmp, tmpb)
                            transpose_to(c, tmpb, nm(tmpb), 64, uT, 'uT', 0, True)
                        else:
                            S.dma('sp', xb[:], I['x_own'][tok0 + s * 128:tok0 + (s + 1) * 128, :], writes=[nm(xb)])
                            modulate(xb, nm(xb), [(0, 128, Ac, Bc) if kind == 'ctx' else (0, 128, Al, Bl)], tmp, tmpb)
                            transpose_to(c, tmpb, nm(tmpb), 128, uT, 'uT', s * 128, True)
                    if kind == 'lat':
                        S.dma('sp', cosT[:], I['cos0'][:, tok0:tok0 + 512], writes=['cos'])
                        S.dma('sp', sinT[:], I['sin0'][:, tok0:tok0 + 512], writes=['sin'])
                    groups = range(2, 6) if kind == 'halo' else range(9)
                    for g in groups:
                        wt = wg[wcount[0] % 2]
                        kw = nm(wt)
                        wcount[0] += 1
                        S.dma('sp', wt[:], Wb['a_w_in'][:, :, g * 512:(g + 1) * 512], reads=['a_w_in_b'], writes=[kw])
                        for cc in range(4):
                            if g == 8 and cc >= 2:
                                break
                            pb = cc % 4
                            for k in range(KD):
                                S.op('pe', lambda e: e.matmul(ps[pb][:, 0:ntok], wt[:, k, cc * 128:(cc + 1) * 128], uT[:, k, 0:ntok],
                                                              start=(k == 0), stop=(k == KD - 1)), reads=[kw, 'uT'], writes=[PS(pb)])
                            pv = ps[pb][:, 0:ntok]
                            if g < 2:
                                S.op('act', lambda e: e.copy(out=gbo[:, cc, 0:ntok], in_=pv), reads=[PS(pb)], writes=['gbo'])
                            elif g < 4:
                                S.op('act', lambda e: e.copy(out=gcT[:, (g - 2) * 4 + cc, 0:ntok], in_=pv), reads=[PS(pb)], writes=['gcT'])
                            elif g < 6:
                                S.op('dve', lambda e: e.tensor_tensor(out=zt[:, cc, 0:ntok], in0=pv, in1=gcT[:, (g - 4) * 4 + cc, 0:ntok],
                                                                      op=ALU.mult), reads=[PS(pb), 'gcT'], writes=['zt'])
                                if kind == 'halo':
                                    S.op('dve', lambda e: e.tensor_tensor(out=zt[:, cc, 0:64], in0=zt[:, cc, 0:64], in1=hmask[:, :],
                                                                          op=ALU.mult), reads=['zt', 'hmask'], writes=['zt'])
                            else:
                                hidx = (g - 6) * 4 + cc
                                gain = gq if g < 8 else gk
                                S.op('act', lambda e: e.activation(out=sq[:, 0:ntok], in_=pv, func=AF.Square), reads=[PS(pb)], writes=['sq'])
                                S.op('pe', lambda e: e.matmul(ps[4][:, 0:ntok], onesm[:], sq[:, 0:ntok], start=True, stop=True),
                                     reads=['sq', 'onesm'], writes=[PS(4)])
                                S.op('act', lambda e: e.activation(out=sd[:, 0:ntok], in_=ps[4][:, 0:ntok], func=AF.Sqrt, bias=float(RMS_EPS), scale=1.0),
                                     reads=[PS(4)], writes=['sd'])
                                S.op('dve', lambda e: e.reciprocal(out=rstd[:, 0:ntok], in_=sd[:, 0:ntok]), reads=['sd'], writes=['rstd'])
                                S.op('dve', lambda e: e.scalar_tensor_tensor(out=qn[:, 0:ntok], in0=pv, scalar=gain[:, 0:1], in1=rstd[:, 0:ntok],
                                                                             op0=ALU.mult, op1=ALU.mult),
                                     reads=[PS(pb), nm(gain), 'rstd'], writes=['qn'])
                                if kind == 'lat':
                                    rope(qn[:, 0:ntok], 'qn', 128, ntok, rT, cosT[:, 0:ntok], sinT[:, 0:ntok], t1,
                                         qko[:, hidx, 0:ntok], 'qko', 5)
                                else:
                                    S.op('act', lambda e: e.copy(out=qko[:, hidx, 0:ntok], in_=qn[:, 0:ntok]), reads=['qn'], writes=['qko'])
                        if g < 2 and kind != 'halo':
                            S.dma('sp', GB[:, g * 4:(g + 1) * 4, tok0:tok0 + ntok], gbo[:, :, 0:ntok], reads=['gbo'], writes=['GB'], join=True)
                        if 4 <= g < 6:
                            zc = slice((g - 4) * 4, (g - 4) * 4 + 4)
                            if kind == 'halo':
                                for (src_c, dst_c) in ((0, 0), (1, SL + 1), (32, SL + 2), (33, SL + 2 + CL + 1)):
                                    S.dma('sp', ZT[:, zc, dst_c:dst_c + 1], zt[:, :, src_c:src_c + 1], reads=['zt'], writes=['ZT'], join=True, allow_slow_non_contiguous=True)
                            else:
                                base = (SL + 3) if kind == 'ctx' else 1
                                S.dma('sp', ZT[:, zc, base + (tok0 - (SL if kind == 'ctx' else 0)):base + (tok0 - (SL if kind == 'ctx' else 0)) + ntok],
                                      zt[:, :, 0:ntok], reads=['zt'], writes=['ZT'], join=True)
                        if g == 8:
                            for s in range(nsub):
                                pb = 2 + s % 2
                                for k in range(KD):
                                    S.op('pe', lambda e: e.matmul(ps[pb][:, 0:256], uT[:, k, s * 128:(s + 1) * 128], wt[:, k, 256:512],
                                                                  start=(k == 0), stop=(k == KD - 1)), reads=[kw, 'uT'], writes=[PS(pb)])
                                S.op('act', lambda e: e.copy(out=vsb[:, s, :], in_=ps[pb][:, 0:256]), reads=[PS(pb)], writes=['vsb'])
                            for s in range(nsub):
                                S.dma('sp', V_own[tok0 + s * 128:tok0 + (s + 1) * 128, :], vsb[:, s, :],
                                      reads=['vsb'], writes=['V_own'], join=True)
                    if kind != 'halo':
                        for h_ in range(8):
                            S.dma('sp', QT[:, h_, tok0:tok0 + ntok], qko[:, h_, 0:ntok], reads=['qko'], writes=['QT'], join=True)
                        for g_ in range(2):
                            S.dma('sp', KT_own[g_ * 128:(g_ + 1) * 128, tok0:tok0 + ntok], qko[:, 8 + g_, 0:ntok],
                                  reads=['qko'], writes=['KT_own'], join=True)
                S.barrier()

        def phase_l0_attn():
            with contextlib.ExitStack() as ph:
                Tl = mk(ph)
                KT_sb = Tl('KT_sb', [128, 2, 2, T], BF16)
                V_sb = Tl('V_sb', [128, 2 * NST, 256], BF16)
                ones_bf = Tl('ones_bf', [128, 128], BF16)
                cw = Tl('cw', [128, 3, 8])
                Qs = Tl('Qs', [128, 8, 512], BF16)
                zt = Tl('ztc', [128, 8, 514], BF16)
                gbt = Tl('gbt', [128, 8, 512], BF16)
                aT = Tl('aT', [128, 16, 512], BF16)
                ct = Tl('ct', [128, 4, 512])
                pT = Tl('pT', [128, 3, 512], BF16)
                rec = Tl('rec', [128, 512])
                S.op('dve', lambda e: e.memset(ones_bf[:], 1.0), writes=['ones_bf'])
                S.dma('sp', cw[:], I['conv_w_t'][:, :, :], writes=['cw'])
                for g_ in range(2):
                    for r_ in range(2):
                        S.dma('sp', KT_sb[:, g_, r_, :], KT_pair[(r_ * 2 + g_) * 128:(r_ * 2 + g_ + 1) * 128, :], reads=['KT_pair'],
                              writes=['KT_sb'], join=(g_ + r_ > 0))
                for c0 in range(2 * NST):
                    S.dma('sp', V_sb[:, c0, :], V_pair[c0 * 128:(c0 + 1) * 128, :], reads=['V_pair'], writes=['V_sb'], join=(c0 > 0))
                sc = 1.0 / math.sqrt(128)
                for (tok0, ntok, cx) in own_tiles:
                    zb = (SL + 2 + (tok0 - SL)) if cx else tok0
                    S.dma('sp', zt[:, :, 0:ntok + 2], ZT0[:, :, zb:zb + ntok + 2], reads=['ZT'], writes=['ztc'])
                    S.dma('sp', gbt[:, :, 0:ntok], GB[:, :, tok0:tok0 + ntok], reads=['GB'], writes=['gbt'])
                    S.dma('sp', Qs[:, :, 0:ntok], QT[:, :, tok0:tok0 + ntok], reads=['QT'], writes=['Qs'])
                    for cch in range(8):
                        en = 'dve'
                        o_ = 0 if cch % 2 == 0 else 2
                        a, b = ct[:, o_, 0:ntok], ct[:, o_ + 1, 0:ntok]
                        ka, kb = ('ct', o_), ('ct', o_ + 1)
                        S.op(en, lambda e: e.tensor_scalar(out=a, in0=zt[:, cch, 0:ntok], scalar1=cw[:, 0, cch:cch + 1], scalar2=None, op0=ALU.mult),
                             reads=['ztc', 'cw'], writes=[ka])
                        S.op(en, lambda e: e.scalar_tensor_tensor(out=b, in0=zt[:, cch, 1:ntok + 1], scalar=cw[:, 1, cch:cch + 1], in1=a,
                                                                  op0=ALU.mult, op1=ALU.add), reads=['ztc', 'cw', ka], writes=[kb])
                        S.op(en, lambda e: e.scalar_tensor_tensor(out=a, in0=zt[:, cch, 2:ntok + 2], scalar=cw[:, 2, cch:cch + 1], in1=b,
                                                                  op0=ALU.mult, op1=ALU.add), reads=['ztc', 'cw', kb], writes=[ka])
                        S.op(en, lambda e: e.tensor_tensor(out=aT[:, cch, 0:ntok], in0=a, in1=gbt[:, cch, 0:ntok], op=ALU.mult),
                             reads=[ka, 'gbt'], writes=['aT'])
                    if cx:
                        kcs = [(rr, SL // 128 + j) for rr in range(2) for j in range(CL // 128)]
                    else:
                        kcs = [(rr, j) for rr in range(2) for j in range(NST)]
                    for h in range(8):
                        g = h // 4
                        so = [[(KT_sb[:, g, rr, j * 128:(j + 1) * 128], Qs[:, h, 0:ntok])] for (rr, j) in kcs]
                        vo = [V_sb[:, rr * NST + j, g * 128:(g + 1) * 128] for (rr, j) in kcs]
                        attn_tile(so, vo, ntok, sc, pT, ones_bf, rec, aT[:, 8 + h, 0:ntok], 'aT', ['KT_sb', 'V_sb', 'Qs'], par=h)
                    S.dma('sp', AT0[:, :, tok0:tok0 + ntok], aT[:, :, 0:ntok], reads=['aT'], writes=['AT0'], join=True)
                S.barrier()

        def phase_post_mixer(l, AT, wname, X_in, X_out, tiles):
            with contextlib.ExitStack() as ph:
                Tl = mk(ph)
                c = load_consts(Tl)
                gs, A, Bv = Tl('gs', [128, D]), Tl('A', [128, D]), Tl('Bv', [128, D])
                lng, lnb = Tl('lng', [128, D]), Tl('lnb', [128, D])
                bcast_row(lng, I['ln_g'][2 * l:2 * l + 1, :], 'lng')
                bcast_row(lnb, I['ln_b'][2 * l:2 * l + 1, :], 'lnb')
                aT = Tl('aT', [128, 16, 512], BF16)
                wo = [Tl('wo%d' % i, [128, KD, 512], BF16) for i in range(2)]
                r = Tl('r', [128, 4, D])
                ys = Tl('ys', [128, 2, 512])
                x1 = Tl('x1', [128, D])
                vv = Tl('vv', [128, D])
                junk = Tl('junk', [128, D])
                st_t = Tl('st_t', [128, 8])
                vT32 = Tl('vT32', [128, KD, 128])
                vTb = Tl('vTb', [128, KD, 512], BF16)
                RW = Tl('RW', [128, KD, NE])
                rb = Tl('rb', [128, NE])
                gt = {n_: Tl('g_' + n_, shp) for n_, shp in [('sc', [128, NE]), ('sel', [128, NE]), ('eq', [128, NE]), ('sel2', [128, NE]),
                                                             ('m1', [128, NG]), ('m2', [128, NG]), ('grp', [128, NG]), ('g8', [128, 8]),
                                                             ('gm', [128, NG]), ('pen', [128, NG]), ('selm', [128, NE]), ('t8', [128, 8]),
                                                             ('em', [128, NE]), ('gw', [128, NE]), ('ss', [128, 1]), ('rs', [128, 1]),
                                                             ('G', [128, NE])]}
                S.dma('sp', RW[:], I['router_w_t'][l], writes=['RW'])
                bcast_row(rb, I['router_b'][l:l + 1, :], 'rb')
                wc = 0
                cur = None
                for ti, (tok0, ntok, cx) in enumerate(tiles):
                    if cur != cx:
                        bcast_param(c, gs, l, 2, cx, scale=1.0 / DN_ALPHA)
                        bcast_param(c, A, l, 4, cx, bias=1.0)
                        bcast_param(c, Bv, l, 3, cx)
                        cur = cx
                    nsub = ntok // 128
                    S.dma('sp', aT[:, :, 0:ntok], AT[:, :, tok0:tok0 + ntok], reads=[nm(AT)], writes=['aT'])
                    for s in range(nsub):
                        S.dma('sp', r[:, s, :], X_in[tok0 + s * 128:tok0 + (s + 1) * 128, :], reads=[nm(X_in)], writes=[('r', s)])
                    q = 0
                    for n in range(4):
                        wt = wo[wc % 2]
                        wc += 1
                        S.dma('sp', wt[:], Wb[wname][:, :, n * 512:(n + 1) * 512], reads=[wname + '_b'], writes=[nm(wt)])
                        for s in range(nsub):
                            pb = q % 4
                            q += 1
                            for k in range(KD):
                                S.op('pe', lambda e: e.matmul(ps[pb][:], aT[:, k, s * 128:(s + 1) * 128], wt[:, k, :],
                                                              start=(k == 0), stop=(k == KD - 1)), reads=['aT', nm(wt)], writes=[PS(pb)])
                            yb = ys[:, q % 2, :]
                            S.op('dve', lambda e: e.tensor_tensor(out=yb, in0=ps[pb][:], in1=gs[:, n * 512:(n + 1) * 512], op=ALU.mult),
                                 reads=[PS(pb), 'gs'], writes=[('ys', q % 2)])
                            rv = r[:, s, n * 512:(n + 1) * 512]
                            S.op('pool', lambda e: e.tensor_tensor(out=rv, in0=rv, in1=yb, op=ALU.add), reads=[('ys', q % 2), ('r', s)], writes=[('r', s)])
                    for s in range(nsub):
                        rows = slice(tok0 + s * 128, tok0 + (s + 1) * 128)
                        layer_norm(r[:, s, :], ('r', s), st_t, junk, lng, lnb, x1)
                        S.dma('sp', X_out[rows, :], x1[:], reads=['x1'], writes=[nm(X_out)], join=True)
                        S.op('dve', lambda e: e.tensor_tensor(out=junk[:], in0=x1[:], in1=A[:], op=ALU.mult), reads=['x1', 'A'], writes=['junk'])
                        S.op('dve', lambda e: e.tensor_tensor(out=vv[:], in0=junk[:], in1=Bv[:], op=ALU.add), reads=['junk', 'Bv'], writes=['vv'])
                        transpose_to(c, vv, 'vv', 128, vT32, 'vT32', 0, False, cast_dst=None)
                        S.op('pool', lambda e: e.tensor_copy(out=vTb[:, :, s * 128:(s + 1) * 128], in_=vT32[:]), reads=['vT32'], writes=['vTb'])
                        for k in range(KD):
                            S.op('pe', lambda e: e.matmul(ps[4][:, 0:NE], vT32[:, k, :], RW[:, k, :], start=(k == 0), stop=(k == KD - 1)),
                                 reads=['vT32', 'RW'], writes=[PS(4)])
                        g3 = lambda t: t[:].rearrange("p (g e) -> p g e", g=NG)
                        S.op('act', lambda e: e.activation(out=gt['sc'][:], in_=ps[4][:, 0:NE], func=AF.Sigmoid), reads=[PS(4)], writes=['g_sc'])
                        S.op('dve', lambda e: e.tensor_tensor(out=gt['sel'][:], in0=gt['sc'][:], in1=rb[:], op=ALU.add), reads=['g_sc', 'rb'], writes=['g_sel'])
                        S.op('dve', lambda e: e.tensor_reduce(out=gt['m1'][:], in_=g3(gt['sel']), axis=AX.X, op=ALU.max), reads=['g_sel'], writes=['g_m1'])
                        S.op('dve', lambda e: e.tensor_tensor(out=g3(gt['eq']), in0=g3(gt['sel']), in1=gt['m1'][:].unsqueeze(2).to_broadcast([128, NG, GS]),
                                                              op=ALU.is_equal), reads=['g_sel', 'g_m1'], writes=['g_eq'])
                        S.op('dve', lambda e: e.scalar_tensor_tensor(out=gt['sel2'][:], in0=gt['eq'][:], scalar=-1e30, in1=gt['sel'][:],
                                                                     op0=ALU.mult, op1=ALU.add), reads=['g_eq', 'g_sel'], writes=['g_sel2'])
                        S.op('dve', lambda e: e.tensor_reduce(out=gt['m2'][:], in_=g3(gt['sel2']), axis=AX.X, op=ALU.max), reads=['g_sel2'], writes=['g_m2'])
                        S.op('dve', lambda e: e.tensor_tensor(out=gt['grp'][:], in0=gt['m1'][:], in1=gt['m2'][:], op=ALU.add), reads=['g_m1', 'g_m2'], writes=['g_grp'])
                        S.op('dve', lambda e: e.max(out=gt['g8'][:], in_=gt['grp'][:]), reads=['g_grp'], writes=['g_g8'])
                        S.op('dve', lambda e: e.tensor_scalar(out=gt['gm'][:], in0=gt['grp'][:], scalar1=gt['g8'][:, TOPG - 1:TOPG], scalar2=None,
                                                              op0=ALU.is_ge), reads=['g_grp', 'g_g8'], writes=['g_gm'])
                        S.op('dve', lambda e: e.tensor_scalar(out=gt['pen'][:], in0=gt['gm'][:], scalar1=-1.0, scalar2=1e30, op0=ALU.add, op1=ALU.mult),
                             reads=['g_gm'], writes=['g_pen'])
                        S.op('dve', lambda e: e.tensor_tensor(out=g3(gt['selm']), in0=g3(gt['sel']), in1=gt['gm'][:].unsqueeze(2).to_broadcast([128, NG, GS]),
                                                              op=ALU.mult), reads=['g_sel', 'g_gm'], writes=['g_selm'])
                        S.op('dve', lambda e: e.tensor_tensor(out=g3(gt['selm']), in0=g3(gt['selm']), in1=gt['pen'][:].unsqueeze(2).to_broadcast([128, NG, GS]),
                                                              op=ALU.add), reads=['g_selm', 'g_pen'], writes=['g_selm'])
                        S.op('dve', lambda e: e.max(out=gt['t8'][:], in_=gt['selm'][:]), reads=['g_selm'], writes=['g_t8'])
                        S.op('dve', lambda e: e.tensor_scalar(out=gt['em'][:], in0=gt['selm'][:], scalar1=gt['t8'][:, TOPK - 1:TOPK], scalar2=None,
                                                              op0=ALU.is_ge), reads=['g_selm', 'g_t8'], writes=['g_em'])
                        S.op('dve', lambda e: e.tensor_tensor(out=gt['gw'][:], in0=gt['sc'][:], in1=gt['em'][:], op=ALU.mult), reads=['g_sc', 'g_em'], writes=['g_gw'])
                        S.op('dve', lambda e: e.tensor_reduce(out=gt['ss'][:], in_=gt['gw'][:], axis=AX.X, op=ALU.add), reads=['g_gw'], writes=['g_ss'])
                        S.op('dve', lambda e: e.reciprocal(out=gt['rs'][:], in_=gt['ss'][:]), reads=['g_ss'], writes=['g_rs'])
                        S.op('dve', lambda e: e.tensor_scalar(out=gt['G'][:], in0=gt['gw'][:], scalar1=gt['rs'][:, 0:1], scalar2=float(ROUTED_SCALE),
                                                              op0=ALU.mult, op1=ALU.mult), reads=['g_gw', 'g_rs'], writes=['g_G'])
                        S.dma('sp', G_own[rows, :], gt['G'][:], reads=['g_G'], writes=['G_own'], join=True)
                    for k_ in range(KD):
                        S.dma('sp', VTo[ti][k_ * 128:(k_ + 1) * 128, :], vTb[:, k_, 0:ntok],
                              reads=['vTb'], writes=['VTo%d' % ti], join=(k_ > 0))
                S.barrier()

        def phase_moe(l, tiles):
            with contextlib.ExitStack() as ph:
                Tl = mk(ph)
                wgt = [Tl('mwg%d' % i, [128, KD, FF], BF16) for i in range(2)]
                wut = [Tl('mwu%d' % i, [128, KD, FF], BF16) for i in range(2)]
                wdt = [Tl('mwd%d' % i, [128, FFC, D], BF16) for i in range(2)]
                vT = [Tl('mvT%d' % i, [128, KD, 512], BF16) for i in range(2)]
                acc = Tl('acc', [128, 4, D])
                hm = Tl('hm', [128, FFC, 512], BF16)
                sg = Tl('sg', [128, 2, 512], BF16)
                Gt = Tl('Gt', [128, 4, NE])
                gsc = Tl('gsc', [128, 4, EPC])
                selx = Tl('selx', [128, EPC, NE])
                gtmp = Tl('gtmp', [128, EPC, NE])
                S.dma('sp', selx[:], I['sel_exp'][:, :, :], writes=['selx'])
                wc = [0]
                vc = [0]

                def load_w(srcg, srcu, srcd):
                    i = wc[0] % 2
                    wc[0] += 1
                    S.dma('sp', wgt[i][:], srcg[0], reads=[srcg[1]], writes=[nm(wgt[i])])
                    S.dma('sp', wut[i][:], srcu[0], reads=[srcu[1]], writes=[nm(wut[i])])
                    S.dma('sp', wdt[i][:], srcd[0], reads=[srcd[1]], writes=[nm(wdt[i])])
                    return (wgt[i], wut[i], wdt[i], nm(wgt[i]), nm(wut[i]), nm(wdt[i]))

                def store_acc(dst_rows_ap, ntok, key):
                    nsub = ntok // 128
                    for s_ in range(nsub):
                        S.dma('sp', dst_rows_ap[s_ * 128:(s_ + 1) * 128, :], acc[:, s_, :],
                              reads=[('acc', s_, n) for n in range(4)], writes=[key], join=True)

                for ti, (tok0, ntok, cx) in enumerate(tiles):
                    v = vT[vc[0] % 2]
                    vc[0] += 1
                    for k_ in range(KD):
                        S.dma('sp', v[:, k_, 0:ntok], VTo[ti][k_ * 128:(k_ + 1) * 128, :], reads=['VTo%d' % ti], writes=[nm(v)], join=(k_ > 0))
                    w = load_w((Wb['s_gate'][l], 's_gate_b'), (Wb['s_up'][l], 's_up_b'), (Wb['s_down'][l], 's_down_b'))
                    ffn_tile(v, nm(v), ntok, w + (None,), 'copy', acc, None, None, hm, sg)
                    store_acc(Fsh[tok0:tok0 + ntok, :], ntok, 'Fsh')
                for ti, (tok0, ntok, cx) in enumerate(tiles):
                    for rr in range(8):
                        nsub = ntok // 128
                        v = vT[vc[0] % 2]
                        vc[0] += 1
                        for k_ in range(KD):
                            S.dma('sp', v[:, k_, 0:ntok], VTa[ti][rr * D + k_ * 128:rr * D + (k_ + 1) * 128, :],
                                  reads=['VTa%d' % ti], writes=[nm(v)], join=(k_ > 0))
                        for s_ in range(nsub):
                            S.dma('sp', Gt[:, s_, :], G_all[rr * T + tok0 + s_ * 128:rr * T + tok0 + (s_ + 1) * 128, :],
                                  reads=['G_all'], writes=['Gt'], join=(s_ > 0))
                        for s in range(nsub):
                            S.op('pool', lambda e: e.tensor_tensor(out=gtmp[:], in0=selx[:], in1=Gt[:, s, :].unsqueeze(1).to_broadcast([128, EPC, NE]),
                                                                   op=ALU.mult), reads=['selx', 'Gt'], writes=['gtmp'])
                            S.op('dve', lambda e: e.tensor_reduce(out=gsc[:, s, :], in_=gtmp[:], axis=AX.X, op=ALU.add), reads=['gtmp'], writes=['gsc'])
                        for ei in range(EPC):
                            w = load_w((Wb['e_gate'][l, ei], 'e_gate_b'), (Wb['e_up'][l, ei], 'e_up_b'), (Wb['e_down'][l, ei], 'e_down_b'))
                            ffn_tile(v, nm(v), ntok, w + (ei,), 'gfirst' if ei == 0 else 'gacc', acc, gsc, 'gsc', hm, sg)
                        store_acc(Pp[ti][rr * ntok:(rr + 1) * ntok, :], ntok, 'Pp%d' % ti)
                    S.cc("ReduceScatter", G8, Pp[ti][:, :], Fo[ti][:, :], reads=['Pp%d' % ti], writes=['Fo%d' % ti])
                S.barrier()

        def phase_final_ln(l, X_in, X_out, tiles, to_out):
            with contextlib.ExitStack() as ph:
                Tl = mk(ph)
                c = load_consts(Tl)
                gs = Tl('gs', [128, D])
                lng, lnb = Tl('lng', [128, D]), Tl('lnb', [128, D])
                bcast_row(lng, I['ln_g'][2 * l + 1:2 * l + 2, :], 'lng')
                bcast_row(lnb, I['ln_b'][2 * l + 1:2 * l + 2, :], 'lnb')
                xs = [Tl('fx%d' % i, [128, D]) for i in range(2)]
                f1 = [Tl('ff%d' % i, [128, D]) for i in range(2)]
                f2 = [Tl('fg%d' % i, [128, D]) for i in range(2)]
                x1 = Tl('x1', [128, D])
                junk = Tl('junk', [128, D])
                st_t = Tl('st_t', [128, 8])
                cur = None
                i = 0
                for ti, (tok0, ntok, cx) in enumerate(tiles):
                    if cur != cx:
                        bcast_param(c, gs, l, 5, cx, scale=1.0 / DN_ALPHA)
                        cur = cx
                    for s in range(ntok // 128):
                        rows = slice(tok0 + s * 128, tok0 + (s + 1) * 128)
                        xb, fa, fb = xs[i % 2], f1[i % 2], f2[i % 2]
                        i += 1
                        S.dma('sp', xb[:], X_in[rows, :], reads=[nm(X_in)], writes=[nm(xb)])
                        S.dma('sp', fa[:], Fo[ti][s * 128:(s + 1) * 128, :], reads=['Fo%d' % ti], writes=[nm(fa)])
                        S.dma('sp', fb[:], Fsh[rows, :], reads=['Fsh'], writes=[nm(fb)])
                        S.op('pool', lambda e: e.tensor_tensor(out=fa[:], in0=fa[:], in1=fb[:], op=ALU.add), reads=[nm(fa), nm(fb)], writes=[nm(fa)])
                        S.op('dve', lambda e: e.tensor_tensor(out=fa[:], in0=fa[:], in1=gs[:], op=ALU.mult), reads=[nm(fa), 'gs'], writes=[nm(fa)])
                        S.op('pool', lambda e: e.tensor_tensor(out=xb[:], in0=xb[:], in1=fa[:], op=ALU.add), reads=[nm(fa), nm(xb)], writes=[nm(xb)])
                        layer_norm(xb[:], nm(xb), st_t, junk, lng, lnb, x1)
                        if to_out:
                            S.dma('sp', out[rows, :], x1[:], reads=['x1'], writes=['out'], join=True)
                        else:
                            S.dma('sp', X_out[rows, :], x1[:], reads=['x1'], writes=[nm(X_out)], join=True)
                S.barrier()

        def phase_l1_proj():
            with contextlib.ExitStack() as ph:
                Tl = mk(ph)
                c = load_consts(Tl)
                Al, Bl, Ac, Bc = Tl('Al', [128, D]), Tl('Bl', [128, D]), Tl('Ac', [128, D]), Tl('Bc', [128, D])
                bcast_param(c, Al, 1, 1, False, bias=1.0)
                bcast_param(c, Bl, 1, 0, False)
                bcast_param(c, Ac, 1, 1, True, bias=1.0)
                bcast_param(c, Bc, 1, 0, True)
                xs = [Tl('xs%d' % i, [128, D]) for i in range(2)]
                tmp = Tl('tmp', [128, D])
                tmpb = Tl('tmpb', [128, D], BF16)
                uT = Tl('uT', [128, KD, 512], BF16)
                Wd = Tl('Wd', [128, KD, 1088], BF16)
                sq = Tl('sq', [128, 4, 512])
                sd = Tl('sd', [128, 512])
                rstd = Tl('rstd', [128, 512])
                cn = Tl('cn', [128, 4, 512], BF16)
                kr32 = Tl('kr32', [64, 512])
                krb = Tl('krb', [64, 512], BF16)
                t1 = Tl('t1', [128, 2, 512])
                cosT, sinT = Tl('cosT', [64, 512]), Tl('sinT', [64, 512])
                rT = Tl('rT', [64, 64])
                onesm = Tl('onesm', [128, 128])
                gq, gkv = Tl('gq', [128, 4]), Tl('gkv', [128, 4])
                S.dma('sp', Wd[:], Wb['m_w_down'][:, :, :], reads=['m_w_down_b'], writes=['Wd'])
                S.dma('sp', rT[:], I['r64t'][:, :], writes=['rT'])
                S.dma('sp', gq[:], I['m_q_gain_t'][:, :], writes=['gq'])
                S.dma('sp', gkv[:], I['m_kv_gain_t'][:, :], writes=['gkv'])
                S.op('dve', lambda e: e.memset(onesm[:], 1.0 / 512), writes=['onesm'])
                xi = 0
                for (tok0, ntok, cx) in own_tiles:
                    for s in range(ntok // 128):
                        xb = xs[xi % 2]
                        xi += 1
                        S.dma('sp', xb[:], X2[tok0 + s * 128:tok0 + (s + 1) * 128, :], reads=['X2'], writes=[nm(xb)])
                        modulate(xb, nm(xb), [(0, 128, Ac, Bc) if cx else (0, 128, Al, Bl)], tmp, tmpb)
                        transpose_to(c, tmpb, nm(tmpb), 128, uT, 'uT', s * 128, True)
                    if not cx:
                        S.dma('sp', cosT[:], I['cos1'][:, tok0:tok0 + 512], writes=['cos'])
                        S.dma('sp', sinT[:], I['sin1'][:, tok0:tok0 + 512], writes=['sin'])
                    for part in ([0, 1] if not cx else [1]):
                        gain = gq if part == 0 else gkv
                        for cc in range(4):
                            col = part * 512 + cc * 128
                            for k in range(KD):
                                S.op('pe', lambda e: e.matmul(ps[cc][:, 0:ntok], Wd[:, k, col:col + 128], uT[:, k, 0:ntok],
                                                              start=(k == 0), stop=(k == KD - 1)), reads=['Wd', 'uT'], writes=[PS(cc)])
                            S.op('act', lambda e: e.activation(out=sq[:, cc, 0:ntok], in_=ps[cc][:, 0:ntok], func=AF.Square),
                                 reads=[PS(cc)], writes=[('sq', cc)])
                        for cc in range(4):
                            S.op('pe', lambda e: e.matmul(ps[4][:, 0:ntok], onesm[:], sq[:, cc, 0:ntok], start=(cc == 0), stop=(cc == 3)),
                                 reads=[('sq', cc), 'onesm'], writes=[PS(4)])
                        S.op('act', lambda e: e.activation(out=sd[:, 0:ntok], in_=ps[4][:, 0:ntok], func=AF.Sqrt, bias=float(RMS_EPS), scale=1.0),
                             reads=[PS(4)], writes=['sd'])
                        S.op('dve', lambda e: e.reciprocal(out=rstd[:, 0:ntok], in_=sd[:, 0:ntok]), reads=['sd'], writes=['rstd'])
                        for cc in range(4):
                            S.op('dve', lambda e: e.scalar_tensor_tensor(out=cn[:, cc, 0:ntok], in0=ps[cc][:, 0:ntok], scalar=gain[:, cc:cc + 1],
                                                                         in1=rstd[:, 0:ntok], op0=ALU.mult, op1=ALU.mult),
                                 reads=[PS(cc), nm(gain), 'rstd'], writes=['cn'])
                        if part == 0:
                            S.dma('sp', CQ[:, :, tok0:tok0 + ntok], cn[:, :, 0:ntok], reads=['cn'], writes=['CQ'], join=True)
                        else:
                            for c_ in range(4):
                                S.dma('sp', KVc_own[c_ // 2][(c_ % 2) * 128:(c_ % 2 + 1) * 128, tok0:tok0 + ntok], cn[:, c_, 0:ntok],
                                      reads=['cn'], writes=['KVc_own%d' % (c_ // 2)], join=True)
                    for k in range(KD):
                        S.op('pe', lambda e: e.matmul(ps[5][0:64, 0:ntok], Wd[:, k, 1024:1088], uT[:, k, 0:ntok],
                                                      start=(k == 0), stop=(k == KD - 1)), reads=['Wd', 'uT'], writes=[PS(5)])
                    if cx:
                        S.op('act', lambda e: e.copy(out=krb[:, 0:ntok], in_=ps[5][0:64, 0:ntok]), reads=[PS(5)], writes=['krb'])
                    else:
                        S.op('act', lambda e: e.copy(out=kr32[:, 0:ntok], in_=ps[5][0:64, 0:ntok]), reads=[PS(5)], writes=['kr32'])
                        rope(kr32[:, 0:ntok], 'kr32', 64, ntok, rT, cosT[:, 0:ntok], sinT[:, 0:ntok], t1, krb[:, 0:ntok], 'krb', 6)
                    S.dma('sp', KR_own[:, tok0:tok0 + ntok], krb[:, 0:ntok], reads=['krb'], writes=['KR_own'], join=True)
                S.barrier()

        def phase_l1_attn():
            with contextlib.ExitStack() as ph:
                Tl = mk(ph)
                ckv = Tl('ckv', [128, 4, 2, T], BF16)
                krs = Tl('krs', [64, 2, T], BF16)
                cqs = Tl('cqs', [128, 4, SL], BF16)
                Wuq = Tl('Wuq', [128, 4, 3072], BF16)
                Wukv = Tl('Wukv', [128, 4, 4096], BF16)
                KnT = Tl('KnT', [128, 2, T], BF16)
                Vh = Tl('Vh', [128, 2 * NST, 128], BF16)
                qn = Tl('qnh', [128, SL], BF16)
                qr = Tl('qrh', [64, SL], BF16)
                qr32 = Tl('qr32', [64, 512])
                t1 = Tl('t1', [128, 2, 512])
                cosT, sinT = Tl('cosT', [64, SL]), Tl('sinT', [64, SL])
                rT = Tl('rT', [64, 64])
                ones_bf = Tl('ones_bf', [128, 128], BF16)
                pT = Tl('pT', [128, 3, 512], BF16)
                rec = Tl('rec', [128, 512])
                ao = Tl('ao', [128, 2, 512], BF16)
                S.op('dve', lambda e: e.memset(ones_bf[:], 1.0), writes=['ones_bf'])
                S.dma('sp', rT[:], I['r64t'][:, :], writes=['rT'])
                S.dma('sp', cosT[:], I['cos1'][:, :], writes=['cos'])
                S.dma('sp', sinT[:], I['sin1'][:, :], writes=['sin'])
                S.dma('sp', Wuq[:], Wb['m_w_uq'][:, :, :], reads=['m_w_uq_b'], writes=['Wuq'])
                S.dma('sp', Wukv[:], Wb['m_w_ukv'][:, :, :], reads=['m_w_ukv_b'], writes=['Wukv'])
                S.dma('sp', cqs[:], CQ[:, :, :], reads=['CQ'], writes=['cqs'])
                for rr in range(2):
                    for c_ in range(4):
                        S.dma('sp', ckv[:, c_, rr, :], KVc_pair[c_ // 2][rr * 256 + (c_ % 2) * 128:rr * 256 + (c_ % 2 + 1) * 128, :],
                              reads=['KVc_pair%d' % (c_ // 2)], writes=['ckv'], join=(rr + c_ > 0))
                    S.dma('sp', krs[:, rr, :], KR_pair[rr * 64:(rr + 1) * 64, :], reads=['KR_pair'], writes=['krs'], join=(rr > 0))
                sc = 1.0 / math.sqrt(192)
                pieces = [(i * 512, 512) for i in range(T // 512)] + ([(T // 512 * 512, T % 512)] if T % 512 else [])
                cnt = 0
                for h in range(16):
                    kc0, vc0 = h * 256, h * 256 + 128
                    for rr in range(2):
                        for (c0, n) in pieces:
                            pb = cnt % 4
                            cnt += 1
                            for k in range(4):
                                S.op('pe', lambda e: e.matmul(ps[pb][:, 0:n], Wukv[:, k, kc0:kc0 + 128], ckv[:, k, rr, c0:c0 + n],
                                                              start=(k == 0), stop=(k == 3)), reads=['Wukv', 'ckv'], writes=[PS(pb)])
                            if cnt % 2:
                                S.op('act', lambda e: e.copy(out=KnT[:, rr, c0:c0 + n], in_=ps[pb][:, 0:n]), reads=[PS(pb)], writes=['KnT'])
                            else:
                                S.op('dve', lambda e: e.tensor_copy(out=KnT[:, rr, c0:c0 + n], in_=ps[pb][:, 0:n]), reads=[PS(pb)], writes=['KnT'])
                        for j0 in range(0, NST, 4):
                            nj = min(4, NST - j0)
                            pb = cnt % 4
                            cnt += 1
                            for jj in range(nj):
                                j = j0 + jj
                                for k in range(4):
                                    S.op('pe', lambda e: e.matmul(ps[pb][:, jj * 128:(jj + 1) * 128], ckv[:, k, rr, j * 128:(j + 1) * 128],
                                                                  Wukv[:, k, vc0:vc0 + 128], start=(k == 0), stop=(k == 3)),
                                         reads=['Wukv', 'ckv'], writes=[PS(pb)])
                            srcv = ps[pb][:, 0:nj * 128].rearrange("p (j v) -> p j v", v=128)
                            dstv = Vh[:, rr * NST + j0:rr * NST + j0 + nj, :]
                            if cnt % 2:
                                S.op('act', lambda e: e.copy(out=dstv, in_=srcv), reads=[PS(pb)], writes=['Vh'])
                            else:
                                S.op('dve', lambda e: e.tensor_copy(out=dstv, in_=srcv), reads=[PS(pb)], writes=['Vh'])
                    for (tok0, ntok, cx) in lat_tiles:
                        pb = cnt % 4
                        cnt += 1
                        for k in range(4):
                            S.op('pe', lambda e: e.matmul(ps[pb][:, 0:ntok], Wuq[:, k, h * 192:h * 192 + 128], cqs[:, k, tok0:tok0 + ntok],
                                                          start=(k == 0), stop=(k == 3)), reads=['Wuq', 'cqs'], writes=[PS(pb)])
                        S.op('act', lambda e: e.copy(out=qn[:, tok0:tok0 + ntok], in_=ps[pb][:, 0:ntok]), reads=[PS(pb)], writes=['qnh'])
                        pb = cnt % 4
                        cnt += 1
                        for k in range(4):
                            S.op('pe', lambda e: e.matmul(ps[pb][0:64, 0:ntok], Wuq[:, k, h * 192 + 128:(h + 1) * 192], cqs[:, k, tok0:tok0 + ntok],
                                                          start=(k == 0), stop=(k == 3)), reads=['Wuq', 'cqs'], writes=[PS(pb)])
                        S.op('act', lambda e: e.copy(out=qr32[:, 0:ntok], in_=ps[pb][0:64, 0:ntok]), reads=[PS(pb)], writes=['qr32'])
                        rope(qr32[:, 0:ntok], 'qr32', 64, ntok, rT, cosT[:, tok0:tok0 + ntok], sinT[:, tok0:tok0 + ntok], t1,
                             qr[:, tok0:tok0 + ntok], 'qrh', (cnt + 1) % 4)
                    for ti, (tok0, ntok, cx) in enumerate(lat_tiles):
                        kcs = [(rr, j) for rr in range(2) for j in range(NST)]
                        so = [[(KnT[:, rr, j * 128:(j + 1) * 128], qn[:, tok0:tok0 + ntok]),
                               (krs[:, rr, j * 128:(j + 1) * 128], qr[:, tok0:tok0 + ntok])] for (rr, j) in kcs]
                        vo = [Vh[:, rr * NST + j, :] for (rr, j) in kcs]
                        ab = ao[:, (h * len(lat_tiles) + ti) % 2, 0:ntok]
                        attn_tile(so, vo, ntok, sc, pT, ones_bf, rec, ab, ('ao', (h * len(lat_tiles) + ti) % 2),
                                  ['KnT', 'Vh', 'qnh', 'qrh', 'krs'], par=h * len(lat_tiles) + ti)
                        S.dma('sp', AT1[:, h, tok0:tok0 + ntok], ab, reads=[('ao', (h * len(lat_tiles) + ti) % 2)], writes=['AT1'], join=True)
                S.barrier()

        def ag_moe(tiles):
            S.cc("AllGather", G8, G_own[:, :], G_all[:, :], reads=['G_own'], writes=['G_all'])
            for ti in range(len(tiles)):
                S.cc("AllGather", G8, VTo[ti][:, :], VTa[ti][:, :], reads=['VTo%d' % ti], writes=['VTa%d' % ti])

        import os
        kstop = int(os.environ.get('K_STOP', '99'))
        steps = []
        def step(fn):
            steps.append(fn)
        step(lambda: phase_l0_proj())
        step(lambda: S.cc("AllGather", G2, KT_own[:, :], KT_pair[:, :], reads=['KT_own'], writes=['KT_pair']))
        step(lambda: S.cc("AllGather", G2, V_own[:, :], V_pair[:, :], reads=['V_own'], writes=['V_pair']))
        step(lambda: phase_l0_attn())
        step(lambda: phase_post_mixer(0, AT0, 'a_w_out', I['x_own'], X1, own_tiles))
        step(lambda: ag_moe(own_tiles))
        step(lambda: phase_moe(0, own_tiles))
        step(lambda: phase_final_ln(0, X1, X2, own_tiles, False))
        step(lambda: phase_l1_proj())
        step(lambda: [S.cc("AllGather", G2, KVc_own[i][:, :], KVc_pair[i][:, :], reads=['KVc_own%d' % i], writes=['KVc_pair%d' % i]) for i in range(2)] +
             [S.cc("AllGather", G2, KR_own[:, :], KR_pair[:, :], reads=['KR_own'], writes=['KR_pair'])])
        step(lambda: phase_l1_attn())
        step(lambda: phase_post_mixer(1, AT1, 'm_w_out', X2, X3, lat_tiles))
        step(lambda: ag_moe(lat_tiles))
        step(lambda: phase_moe(1, lat_tiles))
        step(lambda: phase_final_ln(1, X3, None, lat_tiles, True))
        for i_, fn_ in enumerate(steps):
            if i_ < kstop:
                fn_()
        build.stats = dict(ninst=S.ninst, nsem=S.nalloc + 4)
    return nc


def _rope_tables(n_tok, rot_dim):
    rows = n_tok // GRID_W
    n_freq = rot_dim // 4
    inv = (ROPE_THETA ** (-np.arange(n_freq, dtype=np.float32) / np.float32(n_freq))).astype(np.float32)
    row = np.repeat(np.arange(rows, dtype=np.float32), GRID_W)
    col = np.tile(np.arange(GRID_W, dtype=np.float32), rows)
    ang = np.concatenate([row[:, None] * inv, col[:, None] * inv], axis=-1).astype(np.float32)
    return np.cos(ang).astype(np.float32), np.sin(ang).astype(np.float32)


def _rot_T(n):
    h = n // 2
    R = np.zeros((n, n), np.float32)
    for m in range(h):
        R[m, m + h] = -1.0
        R[m + h, m] = 1.0
    return np.ascontiguousarray(R.T)


def make_in_maps(cfg, inp):
    f = lambda a: np.ascontiguousarray(np.asarray(a, dtype=np.float32))
    SEQ, CTX, NE, FF = cfg.SEQ, cfg.CTX, cfg.NE, cfg.FF
    SL, CL = SEQ // 2, CTX // 2
    EPC = NE // 8
    x, ctx = f(inp['x']), f(inp['ctx'])
    cvec = np.zeros((8, D), np.float32)
    cvec[0:4] = f(inp['c'])
    cvec[4] = f(inp['c_ctx'])
    cos0, sin0 = _rope_tables(SEQ, 128)
    cos1, sin1 = _rope_tables(SEQ, 64)
    shard = lambda w, r: np.ascontiguousarray(w.reshape(-1, w.shape[-1])[r * (w.reshape(-1, w.shape[-1]).shape[0] // 8):(r + 1) * (w.reshape(-1, w.shape[-1]).shape[0] // 8)])
    w_ada = f(inp['w_ada'])
    common = {
        'b_ada': f(inp['b_ada']), 'ln_g': f(inp['ln_g']).reshape(4, D), 'ln_b': f(inp['ln_b']).reshape(4, D),
        'conv_w_t': np.ascontiguousarray(f(inp['a_conv_w'])[0].reshape(3, 8, 128).transpose(2, 0, 1)),
        'a_q_gain': f(inp['a_q_gain'])[0].reshape(128, 1), 'a_k_gain': f(inp['a_k_gain'])[0].reshape(128, 1),
        'm_q_gain_t': np.ascontiguousarray(f(inp['m_q_gain'])[0].reshape(4, 128).T),
        'm_kv_gain_t': np.ascontiguousarray(f(inp['m_kv_gain'])[0].reshape(4, 128).T),
        'router_w_t': np.ascontiguousarray(f(inp['router_w']).reshape(2, KD, 128, NE).transpose(0, 2, 1, 3)),
        'router_b': f(inp['router_b']),
        'r128t': _rot_T(128), 'r64t': _rot_T(64), 'ident': np.eye(128, dtype=np.float32),
    }
    wsh = {'a_w_in_s': f(inp['a_w_in'])[0], 'a_w_out_s': f(inp['a_w_out'])[0], 'm_w_down_s': f(inp['m_w_down'])[0],
           'm_w_uq_s': f(inp['m_w_uq'])[0], 'm_w_ukv_s': f(inp['m_w_ukv'])[0], 'm_w_out_s': f(inp['m_w_out'])[0],
           's_gate_s': f(inp['s_w_gate']), 's_up_s': f(inp['s_w_up']), 's_down_s': f(inp['s_w_down'])}
    eg, eu, ed = f(inp['e_w_gate']), f(inp['e_w_up']), f(inp['e_w_down'])
    maps = []
    for r in range(8):
        b, hf = r // 2, r % 2
        m = dict(common)
        m['x_own'] = np.concatenate([x[b, hf * SL:(hf + 1) * SL], ctx[b, hf * CL:(hf + 1) * CL]], axis=0)
        hx = np.zeros((64, D), np.float32)
        hm = np.zeros((128, 64), np.float32)
        if hf == 1:
            hx[0] = x[b, SL - 1]; hm[:, 0] = 1.0
            hx[32] = ctx[b, CL - 1]; hm[:, 32] = 1.0
        else:
            hx[1] = x[b, SL]; hm[:, 1] = 1.0
            hx[33] = ctx[b, CL]; hm[:, 33] = 1.0
        m['halo_x'], m['halo_mask'] = hx, hm
        m['cT_own'] = np.ascontiguousarray(cvec[:, r * 256:(r + 1) * 256].reshape(8, 2, 128).transpose(2, 1, 0))
        m['w_ada_k'] = np.ascontiguousarray(w_ada[:, r * 256:(r + 1) * 256, :])
        so = np.zeros((6, 128), np.float32); so[b] = 1.0; so[5] = 1.0
        sc = np.zeros((6, 128), np.float32); sc[4] = 1.0; sc[5] = 1.0
        m['sel_own'], m['sel_ctx'] = so, sc
        for k, w in wsh.items():
            m[k] = shard(w, r)
        m['e_gate'] = np.ascontiguousarray(eg[:, r * EPC:(r + 1) * EPC])
        m['e_up'] = np.ascontiguousarray(eu[:, r * EPC:(r + 1) * EPC])
        m['e_down'] = np.ascontiguousarray(ed[:, r * EPC:(r + 1) * EPC])
        pos = slice(hf * SL, (hf + 1) * SL)
        m['cos0'] = np.ascontiguousarray(np.concatenate([cos0[pos].T, cos0[pos].T], axis=0))
        m['sin0'] = np.ascontiguousarray(np.concatenate([sin0[pos].T, sin0[pos].T], axis=0))
        m['cos1'] = np.ascontiguousarray(np.concatenate([cos1[pos].T, cos1[pos].T], axis=0))
        m['sin1'] = np.ascontiguousarray(np.concatenate([sin1[pos].T, sin1[pos].T], axis=0))
        se = np.zeros((128, EPC, NE), np.float32)
        for i in range(EPC):
            se[:, i, r * EPC + i] = 1.0
        m['sel_exp'] = se
        maps.append(m)
    return maps


def run(cfg, inp):
    nc = build(cfg)
    maps = make_in_maps(cfg, inp)
    res = run_bass_kernel_spmd(nc, maps, core_ids=list(range(8)))
    SL = cfg.SEQ // 2
    outs = [res.results[r]['out'] for r in range(8)]
    return np.stack([np.concatenate([outs[2 * b], outs[2 * b + 1]], axis=0) for b in range(4)], axis=0).astype(np.float32)


def kernel(**inputs):
    return run(Cfg(), inputs)
```
